# Optimizing a Trainium2 kernel written in Bass

```python
import functools
import jax, jax.numpy as jnp
from jax import lax
import numpy as np

D_MODEL = 1024
BATCH = 2
SEQ = 8192
DEPTH = 2

GRID_W = 64
CTX_LEN = 256
N_BRANCH = 4
BRANCH_W = 512
EPS = 1e-6

NA_HEADS = 8
NA_HEAD_DIM = BRANCH_W // NA_HEADS
NA_ROWS = 8
NA_COLS = 16

POOL_WINDOWS = (2, 4, 8, 16)
POOL_GROUP = BRANCH_W // len(POOL_WINDOWS)

MLA_HEADS = 8
MLA_Q_LORA = 384
MLA_KV_LORA = 256
MLA_NOPE = 64
MLA_ROPE = 32
MLA_V = BRANCH_W // MLA_HEADS
MLA_BLOCK = 128
ROPE_BASE = 10000.0

CONV_W = BRANCH_W
CONV_K = 3

D_FF = 2816
N_EXPERTS = 8
TOP_K = 2
D_FF_EXPERT = 3584
N_DENSE = (DEPTH + 1) // 2
N_MOE = DEPTH // 2

IN_SIZES = (BRANCH_W, BRANCH_W, BRANCH_W,
            BRANCH_W,
            MLA_Q_LORA, MLA_KV_LORA, MLA_ROPE,
            CONV_W, CONV_W, CONV_W,
            N_BRANCH * D_MODEL)
IN_COLS = sum(IN_SIZES)
IN_SPLITS = tuple(int(s) for s in np.cumsum(IN_SIZES)[:-1])

kernel_name = 'hybrid_parallel_branch_dit_block'


def rms_norm(x, g):
    xf = x.astype(jnp.float32)
    xf = xf * lax.rsqrt(jnp.mean(jnp.square(xf), axis=-1, keepdims=True) + EPS)
    return (xf * g.astype(jnp.float32)).astype(x.dtype)


def modulate(h, shift, scale):
    return h * (1 + scale) + shift


def softmax_f32(s, dtype):
    return jax.nn.softmax(s.astype(jnp.float32), axis=-1).astype(dtype)


def axial_rope_tables(n_tokens):
    pos = jnp.arange(n_tokens)
    row = (pos // GRID_W).astype(jnp.float32)
    col = (pos % GRID_W).astype(jnp.float32)
    n_pairs = MLA_ROPE // 4
    inv_freq = ROPE_BASE ** (-jnp.arange(n_pairs, dtype=jnp.float32) / n_pairs)
    ang = jnp.concatenate([row[:, None] * inv_freq, col[:, None] * inv_freq], axis=-1)
    return jnp.cos(ang), jnp.sin(ang)


def apply_rope(t, cos, sin):
    tp = t.astype(jnp.float32).reshape(t.shape[:-1] + (t.shape[-1] // 2, 2))
    a, b = tp[..., 0], tp[..., 1]
    out = jnp.stack([a * cos - b * sin, a * sin + b * cos], axis=-1)
    return out.reshape(t.shape).astype(t.dtype)


def na_heads(t):
    return t.reshape(t.shape[0], t.shape[1], NA_HEADS, NA_HEAD_DIM)


def neighbourhood_attention(q, k, v, k_ctx, v_ctx, rpb):
    B, S, H, Dh = q.shape
    rows = S // GRID_W
    kr = min(NA_ROWS, rows)
    n_loc = kr * NA_COLS
    scale = Dh ** -0.5
    r = jnp.arange(rows)
    col = jnp.arange(GRID_W)
    key_rows = jnp.clip(r - kr // 2, 0, rows - kr)[:, None] + jnp.arange(kr)
    key_cols = jnp.clip(col - NA_COLS // 2, 0, GRID_W - NA_COLS)[:, None] + jnp.arange(NA_COLS)
    d_row = key_rows - r[:, None] + (NA_ROWS - 1)
    d_col = key_cols - col[:, None] + (NA_COLS - 1)
    bias_col = rpb[:, :, d_col]
    q_rows = q.reshape(B, rows, GRID_W, H, Dh).transpose(1, 0, 2, 3, 4)

    def row_block(args):
        q_r, rows_r, drow_r = args
        idx = (rows_r[None, :, None] * GRID_W + key_cols[:, None, :]).reshape(GRID_W, n_loc)
        k_g = k[:, idx]
        v_g = v[:, idx]
        bias = bias_col[:, drow_r].transpose(0, 2, 1, 3).reshape(H, GRID_W, n_loc)
        s_loc = jnp.einsum('bqhd,bqnhd->bhqn', q_r, k_g) * scale + bias
        s_ctx = jnp.einsum('bqhd,bchd->bhqc', q_r, k_ctx) * scale
        p = softmax_f32(jnp.concatenate([s_loc, s_ctx], axis=-1), v.dtype)
        return (jnp.einsum('bhqn,bqnhd->bqhd', p[..., :n_loc], v_g)
                + jnp.einsum('bhqc,bchd->bqhd', p[..., n_loc:], v_ctx))

    out = lax.map(row_block, (q_rows, key_rows, d_row))
    return out.transpose(1, 0, 2, 3, 4).reshape(B, S, H * Dh)


def context_attention(q, k, v):
    s = jnp.einsum('bqhd,bkhd->bhqk', q, k) * (q.shape[-1] ** -0.5)
    p = softmax_f32(s, v.dtype)
    return jnp.einsum('bhqk,bkhd->bqhd', p, v).reshape(q.shape[0], q.shape[1], -1)


def multiscale_pool(u):
    B, N, _ = u.shape
    uf = u.astype(jnp.float32)
    csum = jnp.pad(jnp.cumsum(uf, axis=1), ((0, 0), (1, 0), (0, 0)))
    t = jnp.arange(N)
    outs = []
    for g, w in enumerate(POOL_WINDOWS):
        lo = jnp.clip(t - w // 2, 0, N)
        hi = jnp.clip(t - w // 2 + w, 0, N)
        sl = slice(g * POOL_GROUP, (g + 1) * POOL_GROUP)
        cnt = (hi - lo).astype(jnp.float32)[None, :, None]
        outs.append((csum[:, hi, sl] - csum[:, lo, sl]) / cnt - uf[:, :, sl])
    return jnp.concatenate(outs, axis=-1).astype(u.dtype)


def pool_branch(u, pool_w, pool_scale):
    B, N, _ = u.shape
    pooled = multiscale_pool(u).reshape(B, N, len(POOL_WINDOWS), POOL_GROUP)
    mixed = jnp.einsum('bngc,gcd->bngd', pooled, pool_w).reshape(B, N, BRANCH_W)
    return mixed * pool_scale


def mla_queries(c_q, g_q, w_uq):
    B, N, _ = c_q.shape
    q = (rms_norm(c_q, g_q) @ w_uq).reshape(B, N, MLA_HEADS, MLA_NOPE + MLA_ROPE)
    return q[..., :MLA_NOPE], q[..., MLA_NOPE:]


def mla_keys_values(c_kv, g_kv, w_ukv):
    B, N, _ = c_kv.shape
    kv = (rms_norm(c_kv, g_kv) @ w_ukv).reshape(B, N, MLA_HEADS, MLA_NOPE + MLA_V)
    return kv[..., :MLA_NOPE], kv[..., MLA_NOPE:]


def mla_scores(qn, qr, kn, kr):
    s = jnp.einsum('bqhd,bkhd->bhqk', qn, kn) + jnp.einsum('bqhr,bkr->bhqk', qr, kr)
    return s * ((MLA_NOPE + MLA_ROPE) ** -0.5)


def mla_latent_attention(qn, qr, kn, kr, v, kn_c, kr_c, v_c):
    B, S, H, _ = qn.shape
    nb = S // MLA_BLOCK

    def block(args):
        qn_b, qr_b = args
        s = jnp.concatenate([mla_scores(qn_b, qr_b, kn, kr), mla_scores(qn_b, qr_b, kn_c, kr_c)], axis=-1)
        p = softmax_f32(s, v.dtype)
        return (jnp.einsum('bhqk,bkhd->bqhd', p[..., :S], v)
                + jnp.einsum('bhqk,bkhd->bqhd', p[..., S:], v_c))

    to_blocks = lambda t: t.reshape((B, nb, MLA_BLOCK) + t.shape[2:]).swapaxes(0, 1)
    out = lax.map(block, (to_blocks(qn), to_blocks(qr)))
    return out.swapaxes(0, 1).reshape(B, S, H * MLA_V)


def mla_context_attention(qn, qr, kn, kr, v):
    p = softmax_f32(mla_scores(qn, qr, kn, kr), v.dtype)
    return jnp.einsum('bhqk,bkhd->bqhd', p, v).reshape(qn.shape[0], qn.shape[1], -1)


def short_conv(u, w):
    return lax.conv_general_dilated(
        u, w[:, None, :].astype(u.dtype), window_strides=(1,),
        padding=((CONV_K // 2, CONV_K // 2),),
        dimension_numbers=('NWC', 'WIO', 'NWC'), feature_group_count=u.shape[-1])


def merge_branches(outs, gate_pre, w_branch):
    o = jnp.stack(outs, axis=-2)
    proj = jnp.einsum('bnko,kod->bnkd', o, w_branch)
    g = jax.nn.sigmoid(gate_pre.reshape(gate_pre.shape[:-1] + (N_BRANCH, D_MODEL)))
    return jnp.sum(g * proj, axis=-2)


def token_mixer(h, hc, cos, sin, w_in, rpb, pool_w, pool_scale, g_q, w_uq, g_kv, w_ukv,
                conv_w, w_branch, w_out, with_ctx_out):
    (q_a, k_a, v_a, u_p, c_q, c_kv, k_r, b_g, c_g, x_c, gate) = jnp.split(h @ w_in, IN_SPLITS, axis=-1)
    (q_ac, k_ac, v_ac, u_pc, c_qc, c_kvc, k_rc, b_gc, c_gc, x_cc, gate_c) = jnp.split(hc @ w_in, IN_SPLITS, axis=-1)
    k_na_c, v_na_c = na_heads(k_ac), na_heads(v_ac)
    kn_c, vm_c = mla_keys_values(c_kvc, g_kv, w_ukv)

    o_a = neighbourhood_attention(na_heads(q_a), na_heads(k_a), na_heads(v_a), k_na_c, v_na_c, rpb)
    o_b = pool_branch(u_p, pool_w, pool_scale)
    qn, qr = mla_queries(c_q, g_q, w_uq)
    kn, vm = mla_keys_values(c_kv, g_kv, w_ukv)
    o_c = mla_latent_attention(qn, apply_rope(qr, cos[:, None], sin[:, None]), kn,
                               apply_rope(k_r, cos, sin), vm, kn_c, k_rc, vm_c)
    o_d = b_g * short_conv(c_g * x_c, conv_w)
    y = merge_branches((o_a, o_b, o_c, o_d), gate, w_branch) @ w_out
    if not with_ctx_out:
        return y, None

    o_ac = context_attention(na_heads(q_ac), k_na_c, v_na_c)
    o_bc = pool_branch(u_pc, pool_w, pool_scale)
    qn_c, qr_c = mla_queries(c_qc, g_q, w_uq)
    o_cc = mla_context_attention(qn_c, qr_c, kn_c, k_rc, vm_c)
    o_dc = b_gc * short_conv(c_gc * x_cc, conv_w)
    yc = merge_branches((o_ac, o_bc, o_cc, o_dc), gate_c, w_branch) @ w_out
    return y, yc


def swiglu(t, w1, w3, w2):
    return (jax.nn.silu(t @ w1) * (t @ w3)) @ w2


def moe_swiglu(t, router, w1, w3, w2):
    logits = (t @ router).astype(jnp.float32)
    top_v, top_i = lax.top_k(logits, TOP_K)
    wts = jax.nn.softmax(top_v, axis=-1)
    combine = jnp.sum(jax.nn.one_hot(top_i, N_EXPERTS, dtype=jnp.float32) * wts[..., None], axis=-2).astype(t.dtype)
    y = jnp.zeros_like(t)
    for e in range(N_EXPERTS):
        y = y + combine[..., e:e + 1] * swiglu(t, w1[e], w3[e], w2[e])
    return y


def setup_inputs(seed: int = 0) -> dict:
    key = jax.random.key(seed)
    ks = jax.random.split(key, 32)
    nrm = lambda k, shape, fan_in: jax.random.normal(k, shape, jnp.float32) * (fan_in ** -0.5)
    gain = lambda k, shape: 1.0 + 0.05 * jax.random.normal(k, shape, jnp.float32)
    return {
        'x': jax.random.normal(ks[0], (BATCH, SEQ, D_MODEL), jnp.float32),
        'c': jax.random.normal(ks[1], (BATCH, D_MODEL), jnp.float32),
        'ctx': jax.random.normal(ks[2], (BATCH, CTX_LEN, D_MODEL), jnp.float32),
        'c_ctx': jax.random.normal(ks[3], (D_MODEL,), jnp.float32),
        'w_ada': 0.5 * nrm(ks[4], (DEPTH, D_MODEL, 6 * D_MODEL), D_MODEL),
        'b_ada': 0.02 * jax.random.normal(ks[5], (DEPTH, 6 * D_MODEL), jnp.float32),
        'g_norm': gain(ks[6], (DEPTH, 4, D_MODEL)),
        'w_in': nrm(ks[7], (DEPTH, D_MODEL, IN_COLS), D_MODEL),
        'na_rpb': 0.1 * jax.random.normal(ks[8], (DEPTH, NA_HEADS, 2 * NA_ROWS - 1, 2 * NA_COLS - 1), jnp.float32),
        'pool_w': nrm(ks[9], (DEPTH, len(POOL_WINDOWS), POOL_GROUP, POOL_GROUP), POOL_GROUP),
        'pool_scale': gain(ks[10], (DEPTH, BRANCH_W)),
        'mla_g_q': gain(ks[11], (DEPTH, MLA_Q_LORA)),
        'mla_w_uq': nrm(ks[12], (DEPTH, MLA_Q_LORA, MLA_HEADS * (MLA_NOPE + MLA_ROPE)), MLA_Q_LORA),
        'mla_g_kv': gain(ks[13], (DEPTH, MLA_KV_LORA)),
        'mla_w_ukv': nrm(ks[14], (DEPTH, MLA_KV_LORA, MLA_HEADS * (MLA_NOPE + MLA_V)), MLA_KV_LORA),
        'conv_w': nrm(ks[15], (DEPTH, CONV_K, CONV_W), CONV_K),
        'w_branch': nrm(ks[16], (DEPTH, N_BRANCH, BRANCH_W, D_MODEL), BRANCH_W),
        'w_out': nrm(ks[17], (DEPTH, D_MODEL, D_MODEL), D_MODEL),
        'ffn_w1': nrm(ks[18], (N_DENSE, D_MODEL, D_FF), D_MODEL),
        'ffn_w3': nrm(ks[19], (N_DENSE, D_MODEL, D_FF), D_MODEL),
        'ffn_w2': nrm(ks[20], (N_DENSE, D_FF, D_MODEL), D_FF),
        'moe_router': nrm(ks[21], (N_MOE, D_MODEL, N_EXPERTS), D_MODEL),
        'moe_w1': nrm(ks[22], (N_MOE, N_EXPERTS, D_MODEL, D_FF_EXPERT), D_MODEL),
        'moe_w3': nrm(ks[23], (N_MOE, N_EXPERTS, D_MODEL, D_FF_EXPERT), D_MODEL),
        'moe_w2': nrm(ks[24], (N_MOE, N_EXPERTS, D_FF_EXPERT, D_MODEL), D_FF_EXPERT),
    }


def reference(x, c, ctx, c_ctx, w_ada, b_ada, g_norm, w_in, na_rpb, pool_w, pool_scale,
              mla_g_q, mla_w_uq, mla_g_kv, mla_w_ukv, conv_w, w_branch, w_out,
              ffn_w1, ffn_w3, ffn_w2, moe_router, moe_w1, moe_w3, moe_w2):
    B, S, _ = x.shape
    cos, sin = axial_rope_tables(S)
    cx = ctx
    for l in range(DEPTH):
        ctx_needed = l < DEPTH - 1
        mod = (jax.nn.silu(c) @ w_ada[l] + b_ada[l]).reshape(B, 6, 1, D_MODEL)
        mod_c = (jax.nn.silu(c_ctx) @ w_ada[l] + b_ada[l]).reshape(6, D_MODEL)

        h = modulate(rms_norm(x, g_norm[l, 0]), mod[:, 0], mod[:, 1])
        hc = modulate(rms_norm(cx, g_norm[l, 0]), mod_c[0], mod_c[1])
        y, yc = token_mixer(h, hc, cos, sin, w_in[l], na_rpb[l], pool_w[l], pool_scale[l],
                            mla_g_q[l], mla_w_uq[l], mla_g_kv[l], mla_w_ukv[l], conv_w[l],
                            w_branch[l], w_out[l], ctx_needed)
        x = x + mod[:, 2] * rms_norm(y, g_norm[l, 1])

        if l % 2 == 0:
            ffn = functools.partial(swiglu, w1=ffn_w1[l // 2], w3=ffn_w3[l // 2], w2=ffn_w2[l // 2])
        else:
            ffn = functools.partial(moe_swiglu, router=moe_router[l // 2], w1=moe_w1[l // 2],
                                    w3=moe_w3[l // 2], w2=moe_w2[l // 2])
        x = x + mod[:, 5] * rms_norm(ffn(modulate(rms_norm(x, g_norm[l, 2]), mod[:, 3], mod[:, 4])), g_norm[l, 3])

        if ctx_needed:
            cx = cx + mod_c[2] * rms_norm(yc, g_norm[l, 1])
            cx = cx + mod_c[5] * rms_norm(ffn(modulate(rms_norm(cx, g_norm[l, 2]), mod_c[3], mod_c[4])), g_norm[l, 3])
    return x
```

```python
import numpy as np
import ml_dtypes
import concourse.bass as bass
import concourse.mybir as mybir
from concourse.bass_utils import run_bass_kernel_spmd

F32 = mybir.dt.float32
BF16 = mybir.dt.bfloat16
AF = mybir.ActivationFunctionType
ALU = mybir.AluOpType
NPBF = ml_dtypes.bfloat16

D = 1024
NCORE = 8
TOWN = 2048
TCTX = 256
T = TOWN + TCTX
NT = T // 128
EPS = 1e-6
IN_COLS = 8352
C_QA, C_KA, C_VA, C_UP, C_CQ, C_CKV, C_KR, C_BG, C_CG, C_XC, C_GATE = (
    0, 512, 1024, 1536, 2048, 2432, 2688, 2720, 3232, 3744, 4256)
TBLOCKS = [(0, 512), (512, 512), (1024, 512), (1536, 512), (2048, 256)]


class Tl:
    def __init__(self, h, name):
        self.h = h
        self.name = name
        self.wr = {}
        self.rd = {}

    def __getitem__(self, idx):
        return V(self, self.h[idx])

    def v(self, ap):
        return V(self, ap)


class V:
    def __init__(self, t, ap):
        self.t = t
        self.ap = ap

    def __getitem__(self, idx):
        return V(self.t, self.ap[idx])


def _aps(x):
    return x.ap if isinstance(x, V) else x


class Ctx:
    def __init__(self, nc, ndma=8):
        self.nc = nc
        self.engs = {"pe": nc.tensor, "act": nc.scalar, "dve": nc.vector, "pool": nc.gpsimd, "sp": nc.sync}
        self.sems = {}
        self.cnt = {}
        for e in ["pe", "act", "dve", "pool"]:
            self.sems[e] = nc.alloc_semaphore("s_" + e)
            self.cnt[e] = 0
        self.dq = {}
        self.dqi = {}
        for q in ["sp", "pool", "act"]:
            ring = []
            for i in range(ndma):
                k = "d_%s%d" % (q, i)
                self.sems[k] = nc.alloc_semaphore(k)
                self.cnt[k] = 0
                ring.append(k)
            self.dq[q] = ring
            self.dqi[q] = 0
        self.seen = {e: {} for e in self.engs}
        self.io = {}
        self.phase = None
        self.nsb = 0
        self.nps = 0
        self.ninst = 0

    def sb(self, shape, dtype, name=None):
        if self.phase is not None:
            return self.phase.sb(shape, dtype, name)
        self.nsb += 1
        name = "%s_%d" % (name or "sb", self.nsb)
        return Tl(self.nc.alloc_sbuf_tensor(name, list(shape), dtype), name)

    def ps(self, shape, dtype=F32, name=None):
        if self.phase is not None:
            return self.phase.ps(shape, dtype, name)
        self.nps += 1
        name = "%s_%d" % (name or "ps", self.nps)
        return Tl(self.nc.alloc_psum_tensor(name, list(shape), dtype), name)

    def dram(self, name, shape, dtype, kind):
        if name in self.io:
            t = self.io[name]
            assert list(t.h.shape) == list(shape), (name, t.h.shape, shape)
            return t
        return self.new_dram(name, shape, dtype, kind)

    def new_dram(self, name, shape, dtype, kind="Internal"):
        return Tl(self.nc.dram_tensor(name, list(shape), dtype, kind=kind).ap(), name)

    def begin_stage(self, io=None):
        self.io = io or {}
        self.phase = None
        ph = Phase(self)
        self.phase = ph
        return ph

    def end_stage(self, ph):
        self.phase = None
        ph.close()
        self.io = {}

    def _need(self, eng, ev):
        if ev is None:
            return
        k, v = ev
        if eng == "pe" and k == "pe":
            return
        if self.seen[eng].get(k, 0) >= v:
            return
        self.engs[eng].wait_ge(self.sems[k], v)
        self.ninst += 1
        self.seen[eng][k] = v

    @staticmethod
    def _dma_append(t, is_dma):
        return is_dma and not t.rd and len(t.wr) > 0 and all(k.startswith("d_") for k in t.wr)

    def _pre(self, eng, reads, writes, is_dma=False):
        for t in reads:
            for k, v in t.wr.items():
                self._need(eng, (k, v))
        for t in writes:
            if not self._dma_append(t, is_dma):
                for k, v in t.wr.items():
                    self._need(eng, (k, v))
            for k, v in t.rd.items():
                self._need(eng, (k, v))

    def _post(self, inst, key, inc, reads, writes, is_dma=False):
        self.cnt[key] += inc
        v = self.cnt[key]
        inst.then_inc(self.sems[key], inc)
        self.ninst += 1
        for t in writes:
            if self._dma_append(t, is_dma):
                t.wr[key] = v
            else:
                t.wr = {key: v}
            t.rd = {}
        for t in reads:
            if t not in writes:
                t.rd[key] = v

    @staticmethod
    def _tiles(vs):
        out = []
        for x in vs:
            if isinstance(x, V) and x.t not in out:
                out.append(x.t)
        return out

    def op(self, eng, fn, reads, writes):
        r = self._tiles(reads)
        w = self._tiles(writes)
        self._pre(eng, r, w)
        inst = fn()
        self._post(inst, eng, 1, r, w)

    def dma(self, q, out, in_, **kw):
        r = self._tiles([in_])
        w = self._tiles([out])
        self._pre(q, r, w, is_dma=True)
        ring = self.dq[q]
        key = ring[self.dqi[q] % len(ring)]
        self.dqi[q] += 1
        inst = self.engs[q].dma_start(out=_aps(out), in_=_aps(in_), **kw)
        self._post(inst, key, 16, r, w, is_dma=True)

    def mm(self, out, lhsT, rhs, start, stop):
        self.op("pe", lambda: self.nc.tensor.matmul(_aps(out), lhsT=_aps(lhsT), rhs=_aps(rhs), start=start, stop=stop),
                [lhsT, rhs], [out])

    def transpose(self, out, in_, ident):
        self.op("pe", lambda: self.nc.tensor.transpose(_aps(out), _aps(in_), _aps(ident)), [in_, ident], [out])

    def act(self, out, in_, func, bias=None, scale=None, accum_out=None, eng="act"):
        kw = {}
        reads = [in_]
        writes = [out]
        if bias is not None:
            kw["bias"] = _aps(bias)
            reads.append(bias)
        if scale is not None:
            kw["scale"] = _aps(scale)
            reads.append(scale)
        if accum_out is not None:
            kw["accum_out"] = _aps(accum_out)
            writes.append(accum_out)
        self.op("act", lambda: self.nc.scalar.activation(out=_aps(out), in_=_aps(in_), func=func, **kw), reads, writes)

    def _ve(self, eng):
        return self.nc.vector if eng == "dve" else self.nc.gpsimd

    def copy(self, out, in_, eng="dve"):
        if eng == "act":
            self.op("act", lambda: self.nc.scalar.copy(out=_aps(out), in_=_aps(in_)), [in_], [out])
        else:
            self.op(eng, lambda: self._ve(eng).tensor_copy(out=_aps(out), in_=_aps(in_)), [in_], [out])

    def tt(self, out, in0, in1, op, eng="dve"):
        self.op(eng, lambda: self._ve(eng).tensor_tensor(out=_aps(out), in0=_aps(in0), in1=_aps(in1), op=op),
                [in0, in1], [out])

    def ts(self, out, in0, s1, s2, op0, op1=None, eng="dve"):
        def fn():
            if op1 is None:
                return self._ve(eng).tensor_scalar(out=_aps(out), in0=_aps(in0), scalar1=_aps(s1), scalar2=None, op0=op0)
            return self._ve(eng).tensor_scalar(out=_aps(out), in0=_aps(in0), scalar1=_aps(s1), scalar2=_aps(s2),
                                               op0=op0, op1=op1)
        self.op(eng, fn, [in0, s1, s2], [out])

    def stt(self, out, in0, scalar, in1, op0, op1, eng="dve"):
        self.op(eng, lambda: self._ve(eng).scalar_tensor_tensor(out=_aps(out), in0=_aps(in0), scalar=_aps(scalar),
                                                                in1=_aps(in1), op0=op0, op1=op1),
                [in0, scalar, in1], [out])

    def recip(self, out, in_):
        self.op("dve", lambda: self.nc.vector.reciprocal(out=_aps(out), in_=_aps(in_)), [in_], [out])

    def memset(self, out, val, eng="dve"):
        self.op(eng, lambda: self._ve(eng).memset(_aps(out), val), [], [out])

    def barrier(self):
        for eng in self.engs:
            for k, v in self.cnt.items():
                if v > 0:
                    self._need(eng, (k, v))

    def finish(self):
        for k, v in self.cnt.items():
            if v > 0:
                self._need("sp", (k, v))


class Phase:
    def __init__(self, cx):
        from contextlib import ExitStack
        self.cx = cx
        self.stack = ExitStack()

    def sb(self, shape, dtype, name=None):
        self.cx.nsb += 1
        nm = "%s_%d" % (name or "ph", self.cx.nsb)
        h = self.stack.enter_context(self.cx.nc.sbuf_tensor(nm, list(shape), dtype))
        return Tl(h, nm)

    def ps(self, shape, dtype=F32, name=None):
        self.cx.nps += 1
        nm = "%s_%d" % (name or "pp", self.cx.nps)
        h = self.stack.enter_context(self.cx.nc.psum_tensor(nm, list(shape), dtype))
        return Tl(h, nm)

    def close(self):
        self.cx.barrier()
        self.stack.close()


def bcast_rows(ap, nparts):
    inner = ap.ap[-1]
    return bass.AP(ap.tensor, ap.offset, [[0, nparts], list(inner)])


def make_identity(cx, dtype=BF16):
    ident = cx.sb([128, 128], dtype, "ident")
    cx.memset(ident[:], 1.0, eng="pool")
    cx.op("pool", lambda: cx.nc.gpsimd.affine_select(out=ident.h[:], in_=ident.h[:], pattern=[[-1, 128]],
                                                     compare_op=ALU.is_equal, fill=0.0, base=0,
                                                     channel_multiplier=1), [ident[:]], [ident[:]])
    return ident


def w_view(ap2d, p=128):
    return ap2d.rearrange("(c p) n -> p c n", p=p)


def emit_mod_pre(cx, js, nv=2):
    res = {}
    for j in js:
        for v in range(nv):
            res[(v, j)] = cx.sb([128, 1024], F32, "mod%d_%d" % (v, j))
    return res


def emit_mod(cx, cin, w_ada, b_ada, js, pss, nv=2, alloc=None, res=None):
    nc = cx.nc
    alloc = alloc or cx.sb
    if res is None:
        res = emit_mod_pre(cx, js, nv)
    craw = alloc([128, 16], F32, "craw")
    cx.dma("sp", craw[:], cin[:])
    csil = alloc([128, 16], F32, "csil")
    cx.act(csil[:], craw[:], AF.Silu)
    crep = alloc([128, 16, 128], BF16, "crep")
    cs = csil.h[:]
    src = bass.AP(cs.tensor, cs.offset, [cs.ap[0], [1, 16], [0, 128]])
    cx.copy(crep[:], csil.v(src))
    wv = w_view(w_ada.h)
    wt = alloc([128, 8, 512], BF16, "wada")
    bb = alloc([128, 1024], F32, "bada")
    for j in js:
        cx.dma("sp", bb[:], b_ada.v(bcast_rows(b_ada.h[j * 1024:(j + 1) * 1024], 128)))
        for hh in range(2):
            cx.dma("pool", wt[:], w_ada.v(wv[:, :, j * 1024 + hh * 512: j * 1024 + (hh + 1) * 512]))
            for v in range(nv):
                o = res[(v, j)]
                pt = pss[(2 * v + hh) % len(pss)]
                for k in range(8):
                    cx.mm(pt[:], crep[:, v * 8 + k, :], wt[:, k, :], k == 0, k == 7)
                cx.tt(o[:, hh * 512:(hh + 1) * 512], pt[:], bb[:, hh * 512:(hh + 1) * 512], ALU.add)
    return res


def emit_mod_into(cx, cin, w_ada, b_ada, js, pss, phase):
    return emit_mod(cx, cin, w_ada, b_ada, js, pss, alloc=phase.sb)


def emit_norm_mod(cx, xt, G, S, out, ss, sd, rs, col, tmp):
    cx.act(tmp[:], xt, AF.Square, accum_out=ss[:, col:col + 1])
    cx.act(sd[:, col:col + 1], ss[:, col:col + 1], AF.Sqrt, bias=None, scale=None)
    return


def build_P(with_q=True, cx=None, io=None):
    own = cx is None
    if own:
        cx = Ctx(bass.Bass("TRN2", target_bir_lowering=False))
    nc = cx.nc
    stage = cx.begin_stage(io)
    xin = cx.dram("xin", [T, D], F32, "ExternalInput")
    cin = cx.dram("cin", [128, 16], F32, "ExternalInput")
    w_ada = cx.dram("w_ada", [D, 6 * D], F32, "ExternalInput")
    b_ada = cx.dram("b_ada", [6 * D], F32, "ExternalInput")
    g0 = cx.dram("g0", [D], F32, "ExternalInput")
    w_in = cx.dram("w_in", [D, IN_COLS], F32, "ExternalInput")
    cosT = cx.dram("cosT", [32, T], F32, "ExternalInput")
    sinT = cx.dram("sinT", [32, T], F32, "ExternalInput")
    rotT = cx.dram("rotT", [32, 32], F32, "ExternalInput")
    outs = {}
    for nm, shp in [("kaT", [512, T]), ("upT", [512, T]), ("zT", [512, T]), ("va", [T, 512]), ("latT", [288, T])]:
        outs[nm] = cx.dram(nm, shp, BF16, "ExternalOutput")
    if with_q:
        for nm, shp in [("qaT", [512, T]), ("bgT", [512, T]), ("cqnT", [384, T]), ("gsT", [4096, T])]:
            outs[nm] = cx.dram(nm, shp, BF16, "ExternalOutput")

    ident = make_identity(cx)
    ones = cx.sb([128, 128], BF16, "ones")
    cx.memset(ones[:], 1.0)
    pss = [cx.ps([128, 512], F32, "pp") for _ in range(4)]
    mods = emit_mod(cx, cin, w_ada, b_ada, [0, 1], pss)
    g0b = cx.sb([128, D], F32, "g0b")
    cx.dma("sp", g0b[:], g0.v(bcast_rows(g0.h[:], 128)))
    G = []
    for v in range(2):
        g = cx.sb([128, D], F32, "G%d" % v)
        cx.stt(g[:], mods[(v, 1)][:], 1.0, g0b[:], ALU.add, ALU.mult)
        G.append(g)
    hT = cx.sb([128, 8, T], BF16, "hT")
    ss = cx.sb([128, NT], F32, "ss")
    cx.memset(ss[:], 0.0)
    sd = cx.sb([128, NT], F32, "sd")
    rs = cx.sb([128, NT], F32, "rs")
    epst = cx.sb([128, 1], F32, "eps")
    cx.memset(epst[:], EPS)
    xts = [cx.sb([128, D], F32, "xt") for _ in range(2)]
    junk = [cx.sb([128, D], F32, "junk") for _ in range(2)]
    hbs = [cx.sb([128, D], BF16, "hb") for _ in range(2)]
    pts = [cx.ps([128, D], BF16, "pT") for _ in range(2)]
    for i in range(NT):
        v = 0 if i < 16 else 1
        xt = xts[i % 2]
        jk = junk[i % 2]
        hb = hbs[i % 2]
        pt = pts[i % 2]
        cx.dma("sp", xt[:], xin[i * 128:(i + 1) * 128, :])
        cx.act(jk[:], xt[:], AF.Square, accum_out=ss[:, i:i + 1])
        cx.act(sd[:, i:i + 1], ss[:, i:i + 1], AF.Sqrt, bias=epst[:], scale=1.0 / D)
        cx.recip(rs[:, i:i + 1], sd[:, i:i + 1])
        cx.stt(jk[:], xt[:], rs[:, i:i + 1], G[v][:], ALU.mult, ALU.mult)
        cx.tt(hb[:], jk[:], mods[(v, 0)][:], ALU.add)
        for c in range(8):
            cx.transpose(pt[:, c * 128:(c + 1) * 128], hb[:, c * 128:(c + 1) * 128], ident[:])
        cx.copy(hT[:, :, i * 128:(i + 1) * 128], pt.v(pt.h[:].rearrange("p (c t) -> p c t", c=8)), eng="act")

    wv = w_view(w_in.h)
    wts = [cx.sb([128, 8, 512], BF16, "wt") for _ in range(2)]
    stg = [cx.sb([128, T], BF16, "stg") for _ in range(3)]
    state = {"g": 0, "p": 0, "s": 0, "e": 0}

    def load_w(c0, n):
        wt = wts[state["g"] % 2]
        state["g"] += 1
        cx.dma("pool", wt[:, :, 0:n], w_in.v(wv[:, :, c0:c0 + n]))
        return wt

    def next_ps():
        p = pss[state["p"] % 4]
        state["p"] += 1
        return p

    def next_stg():
        s = stg[state["s"] % 3]
        state["s"] += 1
        return s

    def proj_fm(wt, j0, m, t0, tn, ps):
        for k in range(8):
            cx.mm(ps[0:m, 0:tn], wt[:, k, j0:j0 + m], hT[:, k, t0:t0 + tn], k == 0, k == 7)

    def evac(out, in_):
        e = "act" if state["e"] % 2 == 0 else "dve"
        state["e"] += 1
        cx.copy(out, in_, eng=e)

    def simple_group(c0, dst, func=None):
        wt = load_w(c0, 512)
        for j in range(4):
            st = next_stg()
            for (t0, tn) in TBLOCKS:
                ps = next_ps()
                proj_fm(wt, j * 128, 128, t0, tn, ps)
                if func is None:
                    evac(st[:, t0:t0 + tn], ps[:, 0:tn])
                else:
                    cx.act(st[:, t0:t0 + tn], ps[:, 0:tn], func)
            cx.dma("sp", dst[j * 128:(j + 1) * 128, :], st[:])

    def normed_group(wt, nch, dst, dim):
        sts = [next_stg() for _ in range(nch)]
        for (t0, tn) in TBLOCKS:
            raw = []
            sqs = []
            for j in range(nch):
                ps = next_ps()
                proj_fm(wt, j * 128, 128, t0, tn, ps)
                r = rawb[j]
                q = sqb[j]
                cx.copy(r[:, 0:tn], ps[:, 0:tn], eng="dve")
                cx.act(q[:, 0:tn], r[:, 0:tn], AF.Square)
                raw.append(r)
                sqs.append(q)
            ps = next_ps()
            for j in range(nch):
                cx.mm(ps[:, 0:tn], ones[:], sqs[j][:, 0:tn], j == 0, j == nch - 1)
            cx.act(sdb[:, 0:tn], ps[:, 0:tn], AF.Sqrt, bias=epst[:], scale=1.0 / dim)
            cx.recip(rsb[:, 0:tn], sdb[:, 0:tn])
            for j in range(nch):
                cx.tt(sts[j][:, t0:t0 + tn], raw[j][:, 0:tn], rsb[:, 0:tn], ALU.mult)
        for j in range(nch):
            cx.dma("sp", dst[j * 128:(j + 1) * 128, :], sts[j][:])

    rawb = [cx.sb([128, 512], F32, "raw") for _ in range(3)]
    sqb = [cx.sb([128, 512], BF16, "sq") for _ in range(3)]
    sdb = cx.sb([128, 512], F32, "sdb")
    rsb = cx.sb([128, 512], F32, "rsb")

    simple_group(C_KA, outs["kaT"])
    simple_group(C_UP, outs["upT"])
    wt = load_w(C_VA, 512)
    vst = [cx.sb([128, 512], BF16, "vst") for _ in range(2)]
    for i in range(NT):
        ps = next_ps()
        for k in range(8):
            cx.mm(ps[:], hT[:, k, i * 128:(i + 1) * 128], wt[:, k, :], k == 0, k == 7)
        evac(vst[i % 2][:], ps[:])
        cx.dma("sp", outs["va"][i * 128:(i + 1) * 128, :], vst[i % 2][:])
    wt = load_w(C_CKV, 288)
    rot_sb = cx.sb([32, 32], BF16, "rot")
    cx.dma("pool", rot_sb[:], rotT[:])
    krb = cx.sb([32, 512], BF16, "krb")
    normed_group(wt, 2, outs["latT"], 256)
    cs_sb = cx.sb([32, 512], F32, "cos")
    sn_sb = cx.sb([32, 512], F32, "sin")
    st = next_stg()
    t1 = cx.sb([32, 512], F32, "t1")
    t2 = cx.sb([32, 512], F32, "t2")
    for (t0, tn) in TBLOCKS:
        pa = next_ps()
        proj_fm(wt, 256, 32, t0, tn, pa)
        cx.copy(krb[:, 0:tn], pa[0:32, 0:tn])
        pb = next_ps()
        cx.mm(pb[0:32, 0:tn], rot_sb[:], krb[:, 0:tn], True, True)
        cx.dma("sp", cs_sb[:, 0:tn], cosT[:, t0:t0 + tn])
        cx.dma("sp", sn_sb[:, 0:tn], sinT[:, t0:t0 + tn])
        cx.tt(t1[:, 0:tn], pa[0:32, 0:tn], cs_sb[:, 0:tn], ALU.mult)
        cx.tt(t2[:, 0:tn], pb[0:32, 0:tn], sn_sb[:, 0:tn], ALU.mult)
        cx.tt(st[0:32, t0:t0 + tn], t1[:, 0:tn], t2[:, 0:tn], ALU.add)
    cx.dma("sp", outs["latT"][256:288, :], st[0:32, :])
    cg = cx.sb([128, 4, T], BF16, "cg")
    wt = load_w(C_CG, 512)
    for j in range(4):
        for (t0, tn) in TBLOCKS:
            ps = next_ps()
            proj_fm(wt, j * 128, 128, t0, tn, ps)
            evac(cg[:, j, t0:t0 + tn], ps[:, 0:tn])
    wt = load_w(C_XC, 512)
    for j in range(4):
        st = next_stg()
        for (t0, tn) in TBLOCKS:
            ps = next_ps()
            proj_fm(wt, j * 128, 128, t0, tn, ps)
            cx.tt(st[:, t0:t0 + tn], ps[:, 0:tn], cg[:, j, t0:t0 + tn], ALU.mult)
        cx.dma("sp", outs["zT"][j * 128:(j + 1) * 128, :], st[:])
    if with_q:
        simple_group(C_QA, outs["qaT"])
        simple_group(C_BG, outs["bgT"])
        wt = load_w(C_CQ, 384)
        normed_group(wt, 3, outs["cqnT"], 384)
        for g in range(8):
            simple_group(C_GATE + g * 512, outs["gsT"][g * 512:(g + 1) * 512, :], func=AF.Sigmoid)
    cx.end_stage(stage)
    if own:
        cx.finish()
    return nc, cx


NKEY = 8192 + TCTX
NKT = NKEY // 128


VW = 768


def vall_views(vall, nkt):
    va = vall.h[:]
    part = va.ap[0]
    ones = bass.AP(va.tensor, va.offset + 64, [part, [VW, nkt], [192, 4], [1, 64]])

    def vdst(kt):
        return bass.AP(va.tensor, va.offset + kt * VW, [part, [192, 4], [128, 2], [1, 64]])
    return ones, vdst


def vall_lhsT(vall, kt, h):
    c0 = (h // 2) * 192 + (h % 2) * 64
    return vall[:, kt, c0:c0 + 128]


def attn_finish(cx, psO, tn, rec, ost, t0, h):
    n0, d0 = (0, 64) if h % 2 == 0 else (64, 0)
    cx.recip(rec[d0:d0 + 64, 0:tn], psO[d0:d0 + 64, 0:tn])
    cx.tt(ost[0:64, t0:t0 + tn], psO[n0:n0 + 64, 0:tn], rec[d0:d0 + 64, 0:tn], ALU.mult)


def build_M(cx=None, io=None):
    own = cx is None
    if own:
        cx = Ctx(bass.Bass("TRN2", target_bir_lowering=False))
    nc = cx.nc
    stage = cx.begin_stage(io)
    cqnT = cx.dram("cqnT", [384, T], BF16, "ExternalInput")
    lat = cx.dram("lat", [288, NKEY], BF16, "ExternalInput")
    wuq = cx.dram("wuq", [384, 768], F32, "ExternalInput")
    gq = cx.dram("gq", [128, 3], F32, "ExternalInput")
    wukv = cx.dram("wukv", [256, 1024], F32, "ExternalInput")
    gkv = cx.dram("gkv", [128, 2], F32, "ExternalInput")
    cq96 = cx.dram("cq96", [96, T], F32, "ExternalInput")
    sq96 = cx.dram("sq96", [96, T], F32, "ExternalInput")
    rot96 = cx.dram("rot96", [96, 96], F32, "ExternalInput")
    ocT = cx.dram("ocT", [512, T], BF16, "ExternalOutput")

    wq = cx.sb([128, 3, 768], BF16, "wq")
    wkv = cx.sb([128, 2, 1024], BF16, "wkv")
    gq_sb = cx.sb([128, 3], F32, "gq")
    gkv_sb = cx.sb([128, 2], F32, "gkv")
    cx.dma("sp", gq_sb[:], gq[:])
    cx.dma("sp", gkv_sb[:], gkv[:])
    for c in range(3):
        cx.dma("pool", wq[:, c, :], wuq[c * 128:(c + 1) * 128, :])
        cx.ts(wq[:, c, :], wq[:, c, :], gq_sb[:, c:c + 1], None, ALU.mult)
    for c in range(2):
        cx.dma("pool", wkv[:, c, :], wukv[c * 128:(c + 1) * 128, :])
        cx.ts(wkv[:, c, :], wkv[:, c, :], gkv_sb[:, c:c + 1], None, ALU.mult)
    rot_sb = cx.sb([96, 96], BF16, "rot")
    cx.dma("pool", rot_sb[:], rot96[:])
    cqt = cx.sb([96, 512], F32, "cqt")
    sqt = cx.sb([96, 512], F32, "sqt")
    cq_sb = cx.sb([128, 3, T], BF16, "cq")
    cx.dma("sp", cq_sb[:], cqnT.v(cqnT.h.rearrange("(c p) t -> p c t", p=128)))
    lat_sb = cx.sb([128, 2, NKEY], BF16, "lat")
    for c in range(2):
        for hh in range(2):
            cx.dma("sp", lat_sb[:, c, hh * 4224:(hh + 1) * 4224], lat[c * 128:(c + 1) * 128, hh * 4224:(hh + 1) * 4224])
    vall = cx.sb([128, NKT, VW], BF16, "vall")
    v_ones, v_dst = vall_views(vall, NKT)
    cx.memset(vall.v(v_ones), 1.0)
    ppj = [cx.ps([128, 512], F32, "pj") for _ in range(2)]
    wv_v = wkv.h[:].rearrange("p c (h two d) -> p c h two d", h=8, two=2)[:, :, :, 1, :]
    for kt in range(NKT):
        ps = ppj[kt % 2]
        for c in range(2):
            cx.mm(ps.v(ps.h[:].rearrange("p (h d) -> p h d", h=8)), lat_sb[:, c, kt * 128:(kt + 1) * 128],
                  wkv.v(wv_v[:, c]), c == 0, c == 1)
        cx.copy(vall.v(v_dst(kt)), ps.v(ps.h[:].rearrange("p (j two d) -> p j two d", j=4, two=2)),
                eng="act" if kt % 2 == 0 else "dve")
    KT = [cx.sb([96, NKEY], BF16, "KT") for _ in range(1)]
    QT = [cx.sb([96, T], BF16, "QT") for _ in range(1)]
    qraw = cx.sb([96, 512], BF16, "qraw")
    t1 = cx.sb([96, 512], F32, "t1")
    t2 = cx.sb([96, 512], F32, "t2")
    pS = [cx.ps([128, 512], F32, "pS") for _ in range(3)]
    pO = [cx.ps([128, 512], F32, "pO") for _ in range(2)]
    PT = [cx.sb([128, 512], BF16, "PT") for _ in range(3)]
    rec = cx.sb([128, 512], F32, "rec")
    osts = [cx.sb([64, T], BF16, "ost") for _ in range(2)]
    nS = 0
    nO = 0
    for h in range(8):
        kT = KT[0]
        qT = QT[0]
        cx.dma("sp", kT[64:96, :], lat[256:288, :])
        kb = 0
        while kb < NKEY:
            kn = min(512, NKEY - kb)
            ps = ppj[(kb // 512) % 2]
            for c in range(2):
                cx.mm(ps[0:64, 0:kn], wkv[:, c, h * 128:h * 128 + 64], lat_sb[:, c, kb:kb + kn], c == 0, c == 1)
            cx.copy(kT[0:64, kb:kb + kn], ps[0:64, 0:kn], eng="act" if (kb // 512) % 2 == 0 else "dve")
            kb += kn
        for (t0, tn) in TBLOCKS:
            pa = ppj[0]
            for c in range(3):
                cx.mm(pa[0:96, 0:tn], wq[:, c, h * 96:(h + 1) * 96], cq_sb[:, c, t0:t0 + tn], c == 0, c == 2)
            cx.copy(qraw[:, 0:tn], pa[0:96, 0:tn], eng="dve")
            pb = ppj[1]
            cx.mm(pb[0:96, 0:tn], rot_sb[:], qraw[:, 0:tn], True, True)
            cx.dma("sp", cqt[:, 0:tn], cq96[:, t0:t0 + tn])
            cx.dma("sp", sqt[:, 0:tn], sq96[:, t0:t0 + tn])
            cx.tt(t1[:, 0:tn], pa[0:96, 0:tn], cqt[:, 0:tn], ALU.mult)
            cx.tt(t2[:, 0:tn], pb[0:96, 0:tn], sqt[:, 0:tn], ALU.mult)
            cx.tt(qT[:, t0:t0 + tn], t1[:, 0:tn], t2[:, 0:tn], ALU.add)
        ost = osts[h % 2]
        for (t0, tn) in TBLOCKS:
            kts = list(range(NKT)) if t0 < TOWN else [NKT - 2, NKT - 1]
            psO = pO[nO % 2]
            nO += 1
            for i, kt in enumerate(kts):
                psS = pS[nS % 3]
                pt = PT[nS % 3]
                nS += 1
                cx.mm(psS[:, 0:tn], kT[:, kt * 128:(kt + 1) * 128], qT[:, t0:t0 + tn], True, True)
                cx.act(pt[:, 0:tn], psS[:, 0:tn], AF.Exp)
                cx.mm(psO[:, 0:tn], vall_lhsT(vall, kt, h), pt[:, 0:tn], i == 0, i == len(kts) - 1)
            attn_finish(cx, psO, tn, rec, ost, t0, h)
        cx.dma("sp", ocT[h * 64:(h + 1) * 64, :], ost[:])
    cx.end_stage(stage)
    if own:
        cx.finish()
    return nc, cx


NEXT = 3072 + TCTX
NKT_N = NEXT // 128


def build_N(cx=None, io=None):
    own = cx is None
    if own:
        cx = Ctx(bass.Bass("TRN2", target_bir_lowering=False))
    nc = cx.nc
    stage = cx.begin_stage(io)
    qaT = cx.dram("qaT", [512, T], BF16, "ExternalInput")
    kaT = cx.dram("kaTx", [512, NEXT], BF16, "ExternalInput")
    vax = cx.dram("vax", [NEXT, 512], BF16, "ExternalInput")
    bias = cx.dram("bias", [8, 4, 12, 128, 512], BF16, "ExternalInput")
    oaT = cx.dram("oaT", [512, T], BF16, "ExternalOutput")
    q_sb = cx.sb([128, 4, T], BF16, "q")
    k_sb = cx.sb([128, 4, NEXT], BF16, "k")
    cx.dma("sp", q_sb[:], qaT.v(qaT.h.rearrange("(c p) t -> p c t", p=128)))
    for c in range(4):
        cx.dma("sp", k_sb[:, c, :], kaT[c * 128:(c + 1) * 128, :])
    vall = cx.sb([128, NKT_N, VW], BF16, "vall")
    v_ones, v_dst = vall_views(vall, NKT_N)
    cx.memset(vall.v(v_ones), 1.0)
    for kt in range(NKT_N):
        src = vax.h[kt * 128:(kt + 1) * 128, :].rearrange("p (j two d) -> p j two d", j=4, two=2)
        for two in range(2):
            cx.dma("sp", vall.v(v_dst(kt)[:, :, two, :]), vax.v(src[:, :, two, :]))
    pS = [cx.ps([128, 512], F32, "pS") for _ in range(3)]
    pO = [cx.ps([128, 512], F32, "pO") for _ in range(2)]
    PT = [cx.sb([128, 512], BF16, "PT") for _ in range(3)]
    SB = [cx.sb([128, 512], F32, "SB") for _ in range(3)]
    BT = [cx.sb([128, 512], BF16, "BT") for _ in range(4)]
    rec = cx.sb([128, 512], F32, "rec")
    osts = [cx.sb([64, T], BF16, "ost") for _ in range(2)]
    nS = 0
    nO = 0
    nB = 0
    for h in range(8):
        p0 = (h % 2) * 64
        c = h // 2
        ost = osts[h % 2]
        for bi, (t0, tn) in enumerate(TBLOCKS):
            if bi < 4:
                kts = [(4 * bi + j, j) for j in range(12)] + [(24, None), (25, None)]
            else:
                kts = [(24, None), (25, None)]
            psO = pO[nO % 2]
            nO += 1
            for i, (kt, bj) in enumerate(kts):
                psS = pS[nS % 3]
                pt = PT[nS % 3]
                sb = SB[nS % 3]
                nS += 1
                cx.mm(psS[:, 0:tn], k_sb[p0:p0 + 64, c, kt * 128:(kt + 1) * 128], q_sb[p0:p0 + 64, c, t0:t0 + tn], True, True)
                if bj is not None:
                    bt = BT[nB % 4]
                    nB += 1
                    cx.dma("sp", bt[:], bias[h, bi, bj])
                    cx.stt(sb[:, 0:tn], psS[:, 0:tn], 0.125, bt[:, 0:tn], ALU.mult, ALU.add)
                    cx.act(pt[:, 0:tn], sb[:, 0:tn], AF.Exp)
                else:
                    cx.act(pt[:, 0:tn], psS[:, 0:tn], AF.Exp, scale=0.125)
                cx.mm(psO[:, 0:tn], vall_lhsT(vall, kt, h), pt[:, 0:tn], i == 0, i == len(kts) - 1)
            attn_finish(cx, psO, tn, rec, ost, t0, h)
        cx.dma("sp", oaT[h * 64:(h + 1) * 64, :], ost[:])
    cx.end_stage(stage)
    if own:
        cx.finish()
    return nc, cx


UPW = 2336
ZW = 2308
SEGS_U = [(8, 0, 2048), (2072, 2048, 256)]
SEGS_Z = [(1, 0, 2048), (2051, 2048, 256)]


def build_C(cx=None, io=None):
    own = cx is None
    if own:
        cx = Ctx(bass.Bass("TRN2", target_bir_lowering=False))
    nc = cx.nc
    stage = cx.begin_stage(io)
    oaT = cx.dram("oaT", [512, T], BF16, "ExternalInput")
    ocT = cx.dram("ocT", [512, T], BF16, "ExternalInput")
    upx = cx.dram("upx", [512, UPW], BF16, "ExternalInput")
    zx = cx.dram("zx", [512, ZW], BF16, "ExternalInput")
    bgT = cx.dram("bgT", [512, T], BF16, "ExternalInput")
    gsT = cx.dram("gsT", [4096, T], BF16, "ExternalInput")
    xin = cx.dram("xin", [T, D], F32, "ExternalInput")
    icnt = cx.dram("icnt", [4, UPW], F32, "ExternalInput")
    pool_w = cx.dram("pool_w", [4, 128, 128], F32, "ExternalInput")
    pscale = cx.dram("pscale", [128, 4], F32, "ExternalInput")
    convw = cx.dram("convw", [128, 12], F32, "ExternalInput")
    w_branch = cx.dram("w_branch", [4, 512, D], F32, "ExternalInput")
    w_out = cx.dram("w_out", [D, D], F32, "ExternalInput")
    g1 = cx.dram("g1", [D], F32, "ExternalInput")
    cin = cx.dram("cin", [128, 16], F32, "ExternalInput")
    w_ada = cx.dram("w_ada", [D, 6 * D], F32, "ExternalInput")
    b_ada = cx.dram("b_ada", [6 * D], F32, "ExternalInput")
    xmid = cx.dram("xmid", [T, D], F32, "ExternalOutput")

    pss = [cx.ps([128, 512], F32, "pp") for _ in range(4)]
    mods = emit_mod(cx, cin, w_ada, b_ada, [2], pss)
    g1b = cx.sb([128, D], F32, "g1b")
    cx.dma("sp", g1b[:], g1.v(bcast_rows(g1.h[:], 128)))
    G2 = []
    for v in range(2):
        g = mods[(v, 2)]
        cx.tt(g[:], g[:], g1b[:], ALU.mult)
        G2.append(g)
    mT = cx.sb([128, 8, T], BF16, "mT")
    P12 = Phase(cx)
    oT = [P12.sb([128, 4, T], BF16, "oT%d" % k) for k in range(4)]
    P1 = Phase(cx)
    cx.dma("sp", oT[0][:], oaT.v(oaT.h.rearrange("(c p) t -> p c t", p=128)))
    cx.dma("sp", oT[2][:], ocT.v(ocT.h.rearrange("(c p) t -> p c t", p=128)))
    ps_sb = P1.sb([128, 4], F32, "pscale")
    cx.dma("sp", ps_sb[:], pscale[:])
    pw = P1.sb([128, 4, 128], BF16, "pw")
    for g in range(4):
        cx.dma("pool", pw[:, g, :], pool_w[g])
    ux = P1.sb([128, UPW], F32, "ux")
    ta = P1.sb([128, UPW], F32, "ta")
    tb_ = P1.sb([128, UPW], F32, "tb")
    ic = P1.sb([128, UPW], F32, "ic")
    pooled = P1.sb([128, T], BF16, "pooled")
    ei = 0
    for g in range(4):
        cx.dma("pool", ux[:], upx[g * 128:(g + 1) * 128, :])
        cx.dma("sp", ic[:], icnt.v(bcast_rows(icnt.h[g], 128)))
        W = UPW
        if g == 0:
            cx.tt(tb_[:, 1:W], ux[:, 0:W - 1], ux[:, 1:W], ALU.add)
            lo, hi = 1, W
        else:
            cx.tt(ta[:, 0:W - 1], ux[:, 0:W - 1], ux[:, 1:W], ALU.add)
            if g == 1:
                cx.tt(tb_[:, 2:W - 1], ta[:, 0:W - 3], ta[:, 2:W - 1], ALU.add)
                lo, hi = 2, W - 1
            else:
                cx.tt(tb_[:, 0:W - 3], ta[:, 0:W - 3], ta[:, 2:W - 1], ALU.add)
                if g == 2:
                    cx.tt(ta[:, 4:W - 3], tb_[:, 0:W - 7], tb_[:, 4:W - 3], ALU.add)
                    lo, hi = 4, W - 3
                else:
                    cx.tt(ta[:, 0:W - 7], tb_[:, 0:W - 7], tb_[:, 4:W - 3], ALU.add)
                    cx.tt(tb_[:, 8:W - 7], ta[:, 0:W - 15], ta[:, 8:W - 7], ALU.add)
                    lo, hi = 8, W - 7
        S = ta if g == 2 else tb_
        other = tb_ if g == 2 else ta
        cx.tt(other[:, lo:hi], S[:, lo:hi], ic[:, lo:hi], ALU.mult)
        for (ec, cc, n) in SEGS_U:
            cx.tt(pooled[:, cc:cc + n], other[:, ec:ec + n], ux[:, ec:ec + n], ALU.subtract)
        for (t0, tn) in TBLOCKS:
            ps = pss[ei % 4]
            ei += 1
            cx.mm(ps[:, 0:tn], pw[:, g, :], pooled[:, t0:t0 + tn], True, True)
            cx.ts(oT[1][:, g, t0:t0 + tn], ps[:, 0:tn], ps_sb[:, g:g + 1], None, ALU.mult)
    cw = P1.sb([128, 12], F32, "cw")
    cx.dma("sp", cw[:], convw[:])
    bg = P1.sb([128, T], F32, "bg")
    for j in range(4):
        cx.dma("pool", ux[:, 0:ZW], zx[j * 128:(j + 1) * 128, :])
        cx.dma("pool", bg[:], bgT[j * 128:(j + 1) * 128, :])
        W = ZW
        cx.ts(ta[:, 1:W - 1], ux[:, 0:W - 2], cw[:, j * 3:j * 3 + 1], None, ALU.mult)
        cx.stt(tb_[:, 1:W - 1], ux[:, 1:W - 1], cw[:, j * 3 + 1:j * 3 + 2], ta[:, 1:W - 1], ALU.mult, ALU.add)
        cx.stt(ta[:, 1:W - 1], ux[:, 2:W], cw[:, j * 3 + 2:j * 3 + 3], tb_[:, 1:W - 1], ALU.mult, ALU.add)
        for (ec, cc, n) in SEGS_Z:
            cx.tt(oT[3][:, j, cc:cc + n], ta[:, ec:ec + n], bg[:, cc:cc + n], ALU.mult)
    P1.close()
    P2 = Phase(cx)
    wbs = [P2.sb([128, 4, 4, 128], BF16, "wb") for _ in range(2)]
    gsb = [P2.sb([128, T], BF16, "gs") for _ in range(4)]
    acc = P2.sb([128, 512], F32, "acc")
    tmp = P2.sb([128, 512], F32, "tmpm")
    ng = 0
    for dc in range(8):
        gk = []
        wbt = wbs[dc % 2]
        for k in range(4):
            for c in range(4):
                cx.dma("pool", wbt[:, k, c, :], w_branch[k, c * 128:(c + 1) * 128, dc * 128:(dc + 1) * 128])
        for k in range(4):
            gt = gsb[k]
            cx.dma("sp", gt[:], gsT[k * 1024 + dc * 128:k * 1024 + (dc + 1) * 128, :])
            gk.append(gt)
        for (t0, tn) in TBLOCKS:
            for k in range(4):
                ps = pss[ei % 4]
                ei += 1
                for c in range(4):
                    cx.mm(ps[:, 0:tn], wbt[:, k, c, :], oT[k][:, c, t0:t0 + tn], c == 0, c == 3)
                if k == 0:
                    cx.tt(acc[:, 0:tn], ps[:, 0:tn], gk[k][:, t0:t0 + tn], ALU.mult)
                else:
                    cx.tt(tmp[:, 0:tn], ps[:, 0:tn], gk[k][:, t0:t0 + tn], ALU.mult)
                    if k < 3:
                        cx.tt(acc[:, 0:tn], acc[:, 0:tn], tmp[:, 0:tn], ALU.add)
                    else:
                        cx.tt(mT[:, dc, t0:t0 + tn], acc[:, 0:tn], tmp[:, 0:tn], ALU.add)
    P2.close()
    P12.close()
    wo = cx.sb([128, 8, D], BF16, "wo")
    for c in range(8):
        cx.dma("pool", wo[:, c, :], w_out[c * 128:(c + 1) * 128, :])
    ss = cx.sb([128, NT], F32, "ss")
    cx.memset(ss[:], 0.0)
    sd = cx.sb([128, NT], F32, "sd")
    rs = cx.sb([128, NT], F32, "rs")
    epst = cx.sb([128, 1], F32, "eps")
    cx.memset(epst[:], EPS)
    ys = [cx.sb([128, D], F32, "y") for _ in range(2)]
    xts = [cx.sb([128, D], F32, "xt") for _ in range(2)]
    jks = [cx.sb([128, D], F32, "jk") for _ in range(2)]
    for i in range(NT):
        v = 0 if i < 16 else 1
        y = ys[i % 2]
        xt = xts[i % 2]
        jk = jks[i % 2]
        cx.dma("sp", xt[:], xin[i * 128:(i + 1) * 128, :])
        for hh in range(2):
            ps = pss[ei % 4]
            ei += 1
            for c in range(8):
                cx.mm(ps[:], mT[:, c, i * 128:(i + 1) * 128], wo[:, c, hh * 512:(hh + 1) * 512], c == 0, c == 7)
            cx.copy(y[:, hh * 512:(hh + 1) * 512], ps[:], eng="act" if hh == 0 else "dve")
        cx.act(jk[:], y[:], AF.Square, accum_out=ss[:, i:i + 1])
        cx.act(sd[:, i:i + 1], ss[:, i:i + 1], AF.Sqrt, bias=epst[:], scale=1.0 / D)
        cx.recip(rs[:, i:i + 1], sd[:, i:i + 1])
        cx.stt(jk[:], y[:], rs[:, i:i + 1], G2[v][:], ALU.mult, ALU.mult)
        cx.tt(y[:], jk[:], xt[:], ALU.add)
        cx.dma("sp", xmid[i * 128:(i + 1) * 128, :], y[:])
    cx.end_stage(stage)
    if own:
        cx.finish()
    return nc, cx


def build_F(moe, cx=None, io=None):
    own = cx is None
    if own:
        cx = Ctx(bass.Bass("TRN2", target_bir_lowering=False))
    nc = cx.nc
    stage = cx.begin_stage(io)
    NE = 8 if moe else 1
    DFF = 3584 if moe else 2816
    xmid = cx.dram("xmid", [T, D], F32, "ExternalInput")
    g2 = cx.dram("g2", [D], F32, "ExternalInput")
    g3 = cx.dram("g3", [D], F32, "ExternalInput")
    cin = cx.dram("cin", [128, 16], F32, "ExternalInput")
    w_ada = cx.dram("w_ada", [D, 6 * D], F32, "ExternalInput")
    b_ada = cx.dram("b_ada", [6 * D], F32, "ExternalInput")
    w1 = cx.dram("w1", [NE, D, DFF], F32, "ExternalInput")
    w3 = cx.dram("w3", [NE, D, DFF], F32, "ExternalInput")
    w2 = cx.dram("w2", [NE, DFF, D], F32, "ExternalInput")
    if moe:
        router = cx.dram("router", [8, D], F32, "ExternalInput")
    xout = cx.dram("xout", [T, D], F32, "ExternalOutput")

    pss = [cx.ps([128, 512], F32, "pp") for _ in range(4)]
    tT = cx.sb([128, 8, T], BF16, "tT")
    yacc = cx.sb([128, NT, D], F32, "yacc")
    cx.memset(yacc[:], 0.0, eng="pool")
    ss = cx.sb([128, 2 * NT], F32, "ss")
    cx.memset(ss[:], 0.0)
    sd = cx.sb([128, 2 * NT], F32, "sd")
    rs = cx.sb([128, 2 * NT], F32, "rs")
    epst = cx.sb([128, 1], F32, "eps")
    cx.memset(epst[:], EPS)
    mods = {}
    for v in range(2):
        for j in [3, 4, 5]:
            mods[(v, j)] = None
    ident = make_identity(cx)
    comb = cx.sb([128, NT, 8], F32, "comb")
    PA0 = Phase(cx)
    PA0.close()
    mods = emit_mod_pre(cx, [3, 4, 5])
    PA = Phase(cx)
    mods = emit_mod(cx, cin, w_ada, b_ada, [3, 4, 5], pss, alloc=PA.sb, res=mods)
    g2b = PA.sb([128, D], F32, "g2b")
    cx.dma("sp", g2b[:], g2.v(bcast_rows(g2.h[:], 128)))
    g3b = PA.sb([128, D], F32, "g3b")
    cx.dma("sp", g3b[:], g3.v(bcast_rows(g3.h[:], 128)))
    Ga, Gb = [], []
    for v in range(2):
        g = mods[(v, 4)]
        cx.stt(g[:], g[:], 1.0, g2b[:], ALU.add, ALU.mult)
        Ga.append(g)
        gg = mods[(v, 5)]
        cx.tt(gg[:], gg[:], g3b[:], ALU.mult)
        Gb.append(gg)
    xts = [PA.sb([128, D], F32, "xt") for _ in range(1)] * 2
    jks = [PA.sb([128, D], F32, "jk") for _ in range(1)] * 2
    tfs = [PA.sb([128, D], F32, "tf") for _ in range(1)] * 2
    hbs = [PA.sb([128, D], BF16, "hb") for _ in range(2)]
    pts = [cx.ps([128, D], BF16, "pT") for _ in range(2)]
    if moe:
        rb = PA.sb([128, 8, D], F32, "rb")
        for e in range(8):
            cx.dma("sp", rb[:, e, :], router.v(bcast_rows(router.h[e], 128)))
        lg = PA.sb([128, NT, 8], F32, "lg")
        cx.memset(lg[:], 0.0)
        sm = PA.sb([128, 8], F32, "sm")
        m1 = PA.sb([128, 1], F32, "m1")
        m2 = PA.sb([128, 1], F32, "m2")
        eq = PA.sb([128, 8], F32, "eq")
        l2 = PA.sb([128, 8], F32, "l2")
        ex = PA.sb([128, 8], F32, "ex")
        den = PA.sb([128, 1], F32, "den")
    for i in range(NT):
        v = 0 if i < 16 else 1
        xt, jk, tf, hb, pt = xts[i % 2], jks[i % 2], tfs[i % 2], hbs[i % 2], pts[i % 2]
        cx.dma("sp", xt[:], xmid[i * 128:(i + 1) * 128, :])
        cx.act(jk[:], xt[:], AF.Square, accum_out=ss[:, i:i + 1])
        cx.act(sd[:, i:i + 1], ss[:, i:i + 1], AF.Sqrt, bias=epst[:], scale=1.0 / D)
        cx.recip(rs[:, i:i + 1], sd[:, i:i + 1])
        cx.stt(jk[:], xt[:], rs[:, i:i + 1], Ga[v][:], ALU.mult, ALU.mult)
        cx.tt(tf[:], jk[:], mods[(v, 3)][:], ALU.add)
        cx.copy(hb[:], tf[:], eng="act")
        for c in range(8):
            cx.transpose(pt[:, c * 128:(c + 1) * 128], hb[:, c * 128:(c + 1) * 128], ident[:])
        cx.copy(tT[:, :, i * 128:(i + 1) * 128], pt.v(pt.h[:].rearrange("p (c t) -> p c t", c=8)), eng="act")
        if moe:
            junk2 = hbs[(i + 1) % 2]
            for e in range(8):
                cx.tt(jk[:], tf[:], rb[:, e, :], ALU.mult)
                cx.act(junk2[:], jk[:], AF.Identity, accum_out=lg[:, i, e:e + 1])
            L = lg[:, i, :]

            def tree8(dst, src, op):
                cx.tt(sm[:, 0:4], src[:, 0:4], src[:, 4:8], op)
                cx.tt(sm[:, 4:6], sm[:, 0:2], sm[:, 2:4], op)
                cx.tt(dst[:, 0:1], sm[:, 4:5], sm[:, 5:6], op)
            tree8(m1, L, ALU.max)
            cx.ts(eq[:], L, m1[:, 0:1], None, ALU.is_equal)
            cx.stt(l2[:], eq[:], -1e30, L, ALU.mult, ALU.add)
            tree8(m2, l2, ALU.max)
            cx.ts(eq[:], L, m2[:, 0:1], None, ALU.is_ge)
            cx.ts(l2[:], L, m1[:, 0:1], None, ALU.subtract)
            cx.act(ex[:], l2[:], AF.Exp)
            cx.tt(ex[:], ex[:], eq[:], ALU.mult)
            tree8(den, ex, ALU.add)
            cx.recip(den[:], den[:])
            cx.ts(comb[:, i, :], ex[:], den[:, 0:1], None, ALU.mult)
    PA.close()
    w1s = [cx.sb([128, 8, 512], BF16, "w1s") for _ in range(2)]
    w3s = [cx.sb([128, 8, 512], BF16, "w3s") for _ in range(2)]
    w2s = [cx.sb([128, 4, D], BF16, "w2s") for _ in range(2)]
    gT = [cx.sb([128, 4, 512], BF16, "gT") for _ in range(2)]
    s1 = [cx.sb([128, 512], BF16, "s1") for _ in range(2)]
    p13 = [cx.ps([128, 512], F32, "p13") for _ in range(2)]
    ng = 0
    nb = 0
    ei = 0
    for e in range(NE):
        f0 = 0
        while f0 < DFF:
            fn = min(512, DFF - f0)
            nfc = fn // 128
            a, b3, c2 = w1s[ng % 2], w3s[ng % 2], w2s[ng % 2]
            ng += 1
            w1v = w_view(w1.h[e])
            w3v = w_view(w3.h[e])
            for k in range(8):
                cx.dma("pool", a[:, k, 0:fn], w1.v(w1v[:, k, f0:f0 + fn]))
                cx.dma("pool", b3[:, k, 0:fn], w3.v(w3v[:, k, f0:f0 + fn]))
            for fc in range(nfc):
                cx.dma("pool", c2[:, fc, :], w2[e, f0 + fc * 128:f0 + (fc + 1) * 128, :])
            for (t0, tn) in TBLOCKS:
                g = gT[nb % 2]
                nb += 1
                for fc in range(nfc):
                    pa, pb = p13[0], p13[1]
                    for k in range(8):
                        cx.mm(pa[:, 0:tn], a[:, k, fc * 128:(fc + 1) * 128], tT[:, k, t0:t0 + tn], k == 0, k == 7)
                    for k in range(8):
                        cx.mm(pb[:, 0:tn], b3[:, k, fc * 128:(fc + 1) * 128], tT[:, k, t0:t0 + tn], k == 0, k == 7)
                    st = s1[fc % 2]
                    cx.act(st[:, 0:tn], pa[:, 0:tn], AF.Silu)
                    cx.tt(g[:, fc, 0:tn], pb[:, 0:tn], st[:, 0:tn], ALU.mult)
                for ti in range(tn // 128):
                    tile = t0 // 128 + ti
                    for hh in range(2):
                        ps = pss[ei % 4]
                        ei += 1
                        for fc in range(nfc):
                            cx.mm(ps[:], g[:, fc, ti * 128:(ti + 1) * 128], c2[:, fc, hh * 512:(hh + 1) * 512], fc == 0, fc == nfc - 1)
                        ya = yacc[:, tile, hh * 512:(hh + 1) * 512]
                        if moe:
                            cx.stt(ya, ps[:], comb[:, tile, e:e + 1], ya, ALU.mult, ALU.add)
                        else:
                            cx.tt(ya, ps[:], ya, ALU.add)
            f0 += fn
    xts = [cx.sb([128, D], F32, "xt") for _ in range(2)]
    jks = [cx.sb([128, D], F32, "jk") for _ in range(2)]
    for i in range(NT):
        v = 0 if i < 16 else 1
        xt, jk = xts[i % 2], jks[i % 2]
        cx.dma("sp", xt[:], xmid[i * 128:(i + 1) * 128, :])
        cx.act(jk[:], yacc[:, i, :], AF.Square, accum_out=ss[:, NT + i:NT + i + 1])
        cx.act(sd[:, NT + i:NT + i + 1], ss[:, NT + i:NT + i + 1], AF.Sqrt, bias=epst[:], scale=1.0 / D)
        cx.recip(rs[:, NT + i:NT + i + 1], sd[:, NT + i:NT + i + 1])
        cx.stt(jk[:], yacc[:, i, :], rs[:, NT + i:NT + i + 1], Gb[v][:], ALU.mult, ALU.mult)
        cx.tt(jk[:], jk[:], xt[:], ALU.add)
        cx.dma("sp", xout[i * 128:(i + 1) * 128, :], jk[:])
    cx.end_stage(stage)
    if own:
        cx.finish()
    return nc, cx


I32 = mybir.dt.int32
GROUPS = [[0, 1, 2, 3], [4, 5, 6, 7]]
BKW = 1024 + 18


def emit_exchange(cx, t, rk):
    nc = cx.nc
    stage = cx.begin_stage()
    kaT, va, latT, upT, zT = t["kaT"], t["va"], t["latT"], t["upT"], t["zT"]
    b_lat, b_k, b_v = t["b_lat"], t["b_k"], t["b_v"]
    g_lat, g_k, g_v, ext = t["g_lat"], t["g_k"], t["g_v"], t["ext"]
    cx.dma("sp", b_lat[:, :], latT[:, 0:TOWN])
    cx.dma("sp", b_k[:, 0:512], kaT[:, 0:512])
    cx.dma("sp", b_k[:, 512:1024], kaT[:, TOWN - 512:TOWN])
    cx.dma("act", b_k[:, 1024:1032], upT[:, 0:8])
    cx.dma("act", b_k[:, 1032:1040], upT[:, TOWN - 8:TOWN])
    cx.dma("act", b_k[:, 1040:1041], zT[:, 0:1], allow_slow_non_contiguous=True)
    cx.dma("act", b_k[:, 1041:1042], zT[:, TOWN - 1:TOWN], allow_slow_non_contiguous=True)
    cx.dma("sp", b_v[0:512, :], va[0:512, :])
    cx.dma("sp", b_v[512:1024, :], va[TOWN - 512:TOWN, :])
    for b_, g_ in ((b_lat, g_lat), (b_k, g_k), (b_v, g_v)):
        r = [b_]
        w = [g_]
        cx._pre("pool", r, w, is_dma=True)
        inst = nc.gpsimd.collective_compute("AllGather", ALU.bypass, replica_groups=GROUPS,
                                            ins=[b_.h[:, :]], outs=[g_.h[:, :]])
        ring = cx.dq["pool"]
        key = ring[cx.dqi["pool"] % len(ring)]
        cx.dqi["pool"] += 1
        cx._post(inst, key, 16, r, w, is_dma=True)
    zt = cx.sb([128, 4, 18], BF16, "zeros")
    cx.memset(zt[:], 0.0)
    cx.dma("sp", ext.v(ext.h[0:512, :].rearrange("(c p) n -> p c n", p=128)), zt[:])
    cx.dma("sp", ext.v(ext.h[2560:3072, :].rearrange("(c p) n -> p c n", p=128)), zt[:])
    cx.dma("sp", ext[512:2560, :], g_k[:, 1024:1042])
    upx, zx, kaTx, vax, lat = t["upx"], t["zx"], t["kaTx"], t["vax"], t["lat"]
    for c0 in (2064, 2328):
        cx.dma("act", upx.v(upx.h[:, c0:c0 + 8].rearrange("(c p) n -> p c n", p=128)), zt[:, :, 0:8])
    for c0 in (2050, 2307):
        cx.dma("act", zx.v(zx.h[:, c0:c0 + 1].rearrange("(c p) n -> p c n", p=128)), zt[:, :, 0:1],
               allow_slow_non_contiguous=True)
    cx.dma("sp", kaTx[:, 512:512 + TOWN], kaT[:, 0:TOWN])
    cx.dma("sp", kaTx[:, 3072:3072 + TCTX], kaT[:, TOWN:T])
    cx.dma("act", vax[512:512 + TOWN, :], va[0:TOWN, :])
    cx.dma("act", vax[3072:3072 + TCTX, :], va[TOWN:T, :])
    cx.dma("sp", upx[:, 8:8 + TOWN], upT[:, 0:TOWN])
    cx.dma("sp", upx[:, 2072:2072 + TCTX], upT[:, TOWN:T])
    cx.dma("act", zx[:, 1:1 + TOWN], zT[:, 0:TOWN])
    cx.dma("act", zx[:, 2051:2051 + TCTX], zT[:, TOWN:T])
    for rr in range(4):
        cx.dma("sp" if rr % 2 == 0 else "act", lat[:, rr * TOWN:(rr + 1) * TOWN], g_lat[rr * 288:(rr + 1) * 288, :])
    cx.dma("sp", lat[:, 4 * TOWN:4 * TOWN + TCTX], latT[:, TOWN:T])
    cx._pre("pool", [g_k, g_v, ext], [])
    pe = [mybir.EngineType.Pool]
    vp = nc.values_load(rk.h[0:1, 0:1], engines=pe, min_val=0, max_val=3)
    vn = nc.values_load(rk.h[0:1, 1:2], engines=pe, min_val=0, max_val=3)
    vpe = nc.values_load(rk.h[0:1, 2:3], engines=pe, min_val=0, max_val=5)
    vne = nc.values_load(rk.h[0:1, 3:4], engines=pe, min_val=0, max_val=5)
    cx.dma("pool", kaTx[:, 0:512], g_k.v(g_k.h[bass.ds(vp * 512, 512), 512:1024]))
    cx.dma("pool", kaTx[:, 512 + TOWN:1024 + TOWN], g_k.v(g_k.h[bass.ds(vn * 512, 512), 0:512]))
    cx.dma("pool", vax[0:512, :], g_v.v(g_v.h[bass.ds(vp * 1024 + 512, 512), :]))
    cx.dma("pool", vax[512 + TOWN:1024 + TOWN, :], g_v.v(g_v.h[bass.ds(vn * 1024, 512), :]))
    extf = ext.h.rearrange("(b r) n -> b (r n)", b=6)
    selp, seln = t["selp"], t["seln"]
    cx.dma("pool", selp[:, :], ext.v(extf[bass.ds(vpe, 1), :]))
    cx.dma("pool", seln[:, :], ext.v(extf[bass.ds(vne, 1), :]))
    sp2 = selp.h.rearrange("o (r n) -> (o r) n", n=18)
    sn2 = seln.h.rearrange("o (r n) -> (o r) n", n=18)
    cx.dma("sp", upx[:, 0:8], selp.v(sp2[:, 8:16]))
    cx.dma("sp", upx[:, 8 + TOWN:16 + TOWN], seln.v(sn2[:, 0:8]))
    cx.dma("sp", zx[:, 0:1], selp.v(sp2[:, 17:18]), allow_slow_non_contiguous=True)
    cx.dma("sp", zx[:, 1 + TOWN:2 + TOWN], seln.v(sn2[:, 16:17]), allow_slow_non_contiguous=True)
    cx.end_stage(stage)


def build_all(nlayers=2):
    cx = Ctx(bass.Bass("TRN2", target_bir_lowering=False))
    nc = cx.nc
    E = lambda name, shape, dt=F32: cx.new_dram(name, shape, dt, "ExternalInput")
    Ii = lambda name, shape, dt=BF16: cx.new_dram(name, shape, dt, "Internal")
    g = {"xin": E("xin", [T, D]), "cin": E("cin", [128, 16]), "rk": E("rk", [1, 4], I32),
         "cosT": E("cosT", [32, T]), "sinT": E("sinT", [32, T]), "rotT": E("rotT", [32, 32]),
         "cq96": E("cq96", [96, T]), "sq96": E("sq96", [96, T]), "rot96": E("rot96", [96, 96]),
         "icnt": E("icnt", [4, UPW])}
    L = []
    for l in range(nlayers):
        moe = (l % 2 == 1)
        NE, DFF = (8, 3584) if moe else (1, 2816)
        d = {"w_ada": E("w_ada%d" % l, [D, 6 * D]), "b_ada": E("b_ada%d" % l, [6 * D]),
             "w_in": E("w_in%d" % l, [D, IN_COLS]), "bias": E("bias%d" % l, [8, 4, 12, 128, 512], BF16),
             "wuq": E("wuq%d" % l, [384, 768]), "gq": E("gq%d" % l, [128, 3]),
             "wukv": E("wukv%d" % l, [256, 1024]), "gkv": E("gkv%d" % l, [128, 2]),
             "pool_w": E("pool_w%d" % l, [4, 128, 128]), "pscale": E("pscale%d" % l, [128, 4]),
             "convw": E("convw%d" % l, [128, 12]), "w_branch": E("w_branch%d" % l, [4, 512, D]),
             "w_out": E("w_out%d" % l, [D, D]),
             "w1": E("w1_%d" % l, [NE, D, DFF]), "w3": E("w3_%d" % l, [NE, D, DFF]), "w2": E("w2_%d" % l, [NE, DFF, D])}
        for j in range(4):
            d["g%d" % j] = E("g%d_%d" % (j, l), [D])
        if moe:
            d["router"] = E("router%d" % l, [8, D])
        L.append(d)
    t = {}
    for nm, shp in [("kaT", [512, T]), ("upT", [512, T]), ("zT", [512, T]), ("va", [T, 512]), ("latT", [288, T]),
                    ("qaT", [512, T]), ("bgT", [512, T]), ("cqnT", [384, T]), ("gsT", [4096, T]),
                    ("b_lat", [288, TOWN]), ("b_k", [512, BKW]), ("b_v", [1024, 512]),
                    ("g_lat", [4 * 288, TOWN]), ("g_k", [4 * 512, BKW]), ("g_v", [4 * 1024, 512]), ("ext", [6 * 512, 18]), ("selp", [1, 512 * 18]), ("seln", [1, 512 * 18]),
                    ("kaTx", [512, NEXT]), ("vax", [NEXT, 512]), ("lat", [288, NKEY]), ("upx", [512, UPW]), ("zx", [512, ZW]),
                    ("oaT", [512, T]), ("ocT", [512, T])]:
        t[nm] = Ii(nm, shp)
    xmid = Ii("xmid", [T, D], F32)
    xs = [g["xin"]] + [Ii("x%d" % (l + 1), [T, D], F32) for l in range(nlayers - 1)]
    xs.append(cx.new_dram("xout", [T, D], F32, "ExternalOutput"))
    for l in range(nlayers):
        d = L[l]
        moe = (l % 2 == 1)
        base = {"cin": g["cin"], "w_ada": d["w_ada"], "b_ada": d["b_ada"]}
        io = dict(base, xin=xs[l], g0=d["g0"], w_in=d["w_in"], cosT=g["cosT"], sinT=g["sinT"], rotT=g["rotT"])
        for nm in ["kaT", "upT", "zT", "va", "latT", "qaT", "bgT", "cqnT", "gsT"]:
            io[nm] = t[nm]
        build_P(True, cx=cx, io=io)
        emit_exchange(cx, t, g["rk"])
        build_N(cx=cx, io={"qaT": t["qaT"], "kaTx": t["kaTx"], "vax": t["vax"], "bias": d["bias"], "oaT": t["oaT"]})
        build_M(cx=cx, io={"cqnT": t["cqnT"], "lat": t["lat"], "wuq": d["wuq"], "gq": d["gq"], "wukv": d["wukv"],
                           "gkv": d["gkv"], "cq96": g["cq96"], "sq96": g["sq96"], "rot96": g["rot96"], "ocT": t["ocT"]})
        build_C(cx=cx, io=dict(base, oaT=t["oaT"], ocT=t["ocT"], upx=t["upx"], zx=t["zx"], bgT=t["bgT"], gsT=t["gsT"],
                               xin=xs[l], icnt=g["icnt"], pool_w=d["pool_w"], pscale=d["pscale"], convw=d["convw"],
                               w_branch=d["w_branch"], w_out=d["w_out"], g1=d["g1"], xmid=xmid))
        iof = dict(base, xmid=xmid, g2=d["g2"], g3=d["g3"], w1=d["w1"], w3=d["w3"], w2=d["w2"], xout=xs[l + 1])
        if moe:
            iof["router"] = d["router"]
        build_F(moe, cx=cx, io=iof)
    cx.finish()
    return nc, cx


def rope_tables():
    pos = np.arange(8192)
    row = (pos // 64).astype(np.float32)
    col = (pos % 64).astype(np.float32)
    inv = (10000.0 ** (-np.arange(8, dtype=np.float32) / 8)).astype(np.float32)
    ang = np.concatenate([row[:, None] * inv, col[:, None] * inv], axis=-1)
    cos = np.repeat(np.cos(ang), 2, axis=1).astype(np.float32)
    sin = np.repeat(np.sin(ang), 2, axis=1).astype(np.float32)
    return cos, sin


def rot_matrix_T(n=32):
    R = np.zeros((n, n), np.float32)
    for i in range(n // 2):
        R[2 * i, 2 * i + 1] = -1.0
        R[2 * i + 1, 2 * i] = 1.0
    return np.ascontiguousarray(R.T)


def cin_layout(vecs):
    out = np.zeros((128, 16), np.float32)
    for v, vec in enumerate(vecs):
        out[:, v * 8:(v + 1) * 8] = np.asarray(vec, np.float32).reshape(8, 128).T
    return out


_CACHE = {}


def get_prog(name, builder):
    if name not in _CACHE:
        _CACHE[name] = builder()
    return _CACHE[name]


def run_P(l, xs, cxs, inputs, with_q=True):
    nc, _ = get_prog("P%d" % int(with_q), lambda: build_P(with_q))
    cos, sin = rope_tables()
    in_maps = []
    for k in range(NCORE):
        b, r = k // 4, k % 4
        cT = np.ones((32, T), np.float32)
        sT = np.zeros((32, T), np.float32)
        cT[:, :TOWN] = cos[r * TOWN:(r + 1) * TOWN].T
        sT[:, :TOWN] = sin[r * TOWN:(r + 1) * TOWN].T
        in_maps.append({
            "xin": np.ascontiguousarray(np.concatenate([xs[k], cxs[b]], axis=0)),
            "cin": cin_layout([inputs["c"][b], inputs["c_ctx"]]),
            "w_ada": np.ascontiguousarray(inputs["w_ada"][l]),
            "b_ada": np.ascontiguousarray(inputs["b_ada"][l]),
            "g0": np.ascontiguousarray(inputs["g_norm"][l, 0]),
            "w_in": np.ascontiguousarray(inputs["w_in"][l]),
            "cosT": cT, "sinT": sT, "rotT": rot_matrix_T(),
        })
    res = run_bass_kernel_spmd(nc, in_maps, core_ids=list(range(NCORE)))
    return res.results


def _run(name, builder, in_maps):
    nc, _ = get_prog(name, builder)
    res = run_bass_kernel_spmd(nc, in_maps, core_ids=list(range(NCORE)))
    return res.results


def na_bias_tables(rpb, rank):
    bi = np.arange(4)[:, None, None, None]
    j = np.arange(12)[None, :, None, None]
    par = np.arange(2)[None, None, :, None]
    qi = np.arange(8)[None, None, None, :]
    q = 32 * rank + 8 * bi + qi
    rho = 32 * rank + 8 * bi - 8 + 2 * j + par
    kr0 = np.clip(q - 4, 0, 120)
    rowvalid = (rho >= 0) & (rho < 128) & (rho >= kr0) & (rho < kr0 + 8)
    drow = np.clip(rho - q + 7, 0, 14)
    kc = np.arange(64)[:, None]
    qc = np.arange(64)[None, :]
    kc0 = np.clip(qc - 8, 0, 48)
    colvalid = (kc >= kc0) & (kc < kc0 + 16)
    dcol = np.clip(kc - qc + 15, 0, 30)
    valid = rowvalid[:, :, :, None, :, None] & colvalid[None, None, None, :, None, :]
    out = np.empty((8, 4, 12, 128, 512), NPBF)
    for h in range(8):
        val = rpb[h][drow[:, :, :, None, :, None], dcol[None, None, None, :, None, :]]
        val = np.where(valid, val, np.float32(-30000.0))
        out[h] = val.reshape(4, 12, 128, 512).astype(NPBF)
    return out


def pool_icnt(rank):
    ic = np.zeros((4, UPW), np.float32)
    for g, w in enumerate((2, 4, 8, 16)):
        for (n, base, off) in ((8192, rank * TOWN, 8), (256, 0, 2072)):
            cnt_n = TOWN if n == 8192 else 256
            pos = base + np.arange(cnt_n)
            lo = np.clip(pos - w // 2, 0, n)
            hi = np.clip(pos - w // 2 + w, 0, n)
            ic[g, off:off + cnt_n] = 1.0 / (hi - lo).astype(np.float32)
    return ic


def kernel(**inputs):
    inputs = {k: np.asarray(v) for k, v in inputs.items()}
    x = inputs["x"]
    xs = [np.ascontiguousarray(x[k // 4, (k % 4) * TOWN:(k % 4 + 1) * TOWN]) for k in range(NCORE)]
    cxs = [np.ascontiguousarray(inputs["ctx"][b]) for b in range(2)]
    cos, sin = rope_tables()
    sc = np.float32(96 ** -0.5)
    rot96 = np.zeros((96, 96), np.float32)
    rot96[64:, 64:] = rot_matrix_T(32)
    zb = lambda *shape: np.zeros(shape, NPBF)
    for l in range(2):
        rp = run_P(l, xs, cxs, inputs)
        cins = [cin_layout([inputs["c"][k // 4], inputs["c_ctx"]]) for k in range(NCORE)]
        w_ada = np.ascontiguousarray(inputs["w_ada"][l])
        b_ada = np.ascontiguousarray(inputs["b_ada"][l])
        maps = []
        for k in range(NCORE):
            r = k % 4
            ka, va = rp[k]["kaT"], rp[k]["va"]
            kp = rp[k - 1]["kaT"][:, 1536:2048] if r > 0 else zb(512, 512)
            kn = rp[k + 1]["kaT"][:, 0:512] if r < 3 else zb(512, 512)
            vp = rp[k - 1]["va"][1536:2048] if r > 0 else zb(512, 512)
            vn = rp[k + 1]["va"][0:512] if r < 3 else zb(512, 512)
            maps.append({
                "qaT": rp[k]["qaT"],
                "kaTx": np.ascontiguousarray(np.concatenate([kp, ka[:, :TOWN], kn, ka[:, TOWN:]], axis=1)),
                "vax": np.ascontiguousarray(np.concatenate([vp, va[:TOWN], vn, va[TOWN:]], axis=0)),
                "bias": na_bias_tables(inputs["na_rpb"][l], r),
            })
        rn = _run("N", build_N, maps)
        maps = []
        gq = np.ascontiguousarray(inputs["mla_g_q"][l].reshape(3, 128).T)
        gkv = np.ascontiguousarray(inputs["mla_g_kv"][l].reshape(2, 128).T)
        for k in range(NCORE):
            b, r = k // 4, k % 4
            lat = np.concatenate([rp[4 * b + rr]["latT"][:, :TOWN] for rr in range(4)] + [rp[4 * b]["latT"][:, TOWN:]], axis=1)
            cq = np.full((96, T), sc, np.float32)
            sq = np.zeros((96, T), np.float32)
            cq[64:, :TOWN] = cos[r * TOWN:(r + 1) * TOWN].T * sc
            sq[64:, :TOWN] = sin[r * TOWN:(r + 1) * TOWN].T * sc
            maps.append({
                "cqnT": rp[k]["cqnT"], "lat": np.ascontiguousarray(lat),
                "wuq": np.ascontiguousarray(inputs["mla_w_uq"][l]), "gq": gq,
                "wukv": np.ascontiguousarray(inputs["mla_w_ukv"][l]), "gkv": gkv,
                "cq96": cq, "sq96": sq, "rot96": rot96,
            })
        rm = _run("M", build_M, maps)
        maps = []
        pscale = np.ascontiguousarray(inputs["pool_scale"][l].reshape(4, 128).T)
        convw = np.ascontiguousarray(inputs["conv_w"][l].reshape(3, 4, 128).transpose(2, 1, 0).reshape(128, 12))
        for k in range(NCORE):
            b, r = k // 4, k % 4
            up, z = rp[k]["upT"], rp[k]["zT"]
            upp = rp[k - 1]["upT"][:, 2040:2048] if r > 0 else zb(512, 8)
            upn = rp[k + 1]["upT"][:, 0:8] if r < 3 else zb(512, 8)
            zp = rp[k - 1]["zT"][:, 2047:2048] if r > 0 else zb(512, 1)
            zn = rp[k + 1]["zT"][:, 0:1] if r < 3 else zb(512, 1)
            maps.append({
                "oaT": rn[k]["oaT"], "ocT": rm[k]["ocT"],
                "upx": np.ascontiguousarray(np.concatenate([upp, up[:, :TOWN], upn, zb(512, 8), up[:, TOWN:], zb(512, 8)], axis=1)),
                "zx": np.ascontiguousarray(np.concatenate([zp, z[:, :TOWN], zn, zb(512, 1), z[:, TOWN:], zb(512, 1)], axis=1)),
                "bgT": rp[k]["bgT"], "gsT": rp[k]["gsT"],
                "xin": np.ascontiguousarray(np.concatenate([xs[k], cxs[b]], axis=0)),
                "icnt": pool_icnt(r),
                "pool_w": np.ascontiguousarray(inputs["pool_w"][l]), "pscale": pscale, "convw": convw,
                "w_branch": np.ascontiguousarray(inputs["w_branch"][l]), "w_out": np.ascontiguousarray(inputs["w_out"][l]),
                "g1": np.ascontiguousarray(inputs["g_norm"][l, 1]), "cin": cins[k], "w_ada": w_ada, "b_ada": b_ada,
            })
        rc = _run("C", build_C, maps)
        maps = []
        moe = (l % 2 == 1)
        if moe:
            w1, w3, w2 = (np.ascontiguousarray(inputs[n][l // 2]) for n in ("moe_w1", "moe_w3", "moe_w2"))
            router = np.ascontiguousarray(inputs["moe_router"][l // 2].T)
        else:
            w1, w3, w2 = (np.ascontiguousarray(inputs[n][l // 2][None]) for n in ("ffn_w1", "ffn_w3", "ffn_w2"))
        for k in range(NCORE):
            m = {"xmid": rc[k]["xmid"], "g2": np.ascontiguousarray(inputs["g_norm"][l, 2]),
                 "g3": np.ascontiguousarray(inputs["g_norm"][l, 3]), "cin": cins[k], "w_ada": w_ada, "b_ada": b_ada,
                 "w1": w1, "w3": w3, "w2": w2}
            if moe:
                m["router"] = router
            maps.append(m)
        rf = _run("F%d" % int(moe), lambda: build_F(moe), maps)
        xs = [np.ascontiguousarray(rf[k]["xout"][:TOWN]) for k in range(NCORE)]
        cxs = [np.ascontiguousarray(rf[4 * b]["xout"][TOWN:]) for b in range(2)]
        _DBG["l%d" % l] = dict(rp=rp, rn=rn, rm=rm, rc=rc, rf=rf) if _DBG.get("on") else None
    out = np.empty_like(x)
    for k in range(NCORE):
        out[k // 4, (k % 4) * TOWN:(k % 4 + 1) * TOWN] = xs[k]
    return out


_DBG = {}
```
